# Optimizing a Trainium2 kernel written in Bass

```python
import jax
import jax.numpy as jnp
from jax import lax
import numpy as np

D_MODEL = 2048
BATCH = 2
SEQ = 8192
DEPTH = 2

HEAD_DIM = 128
ROPE_THETA = 10000.0
NORM_EPS = 1e-6
Q_BLOCK = 128
GATHER_Q_BLOCK = 64

MLA_HEADS = 8
MLA_Q_RANK = 512
MLA_KV_RANK = 256
MLA_NOPE = 128
MLA_ROPE = 64
MLA_V = 128

DSA_HEADS = 8
IDX_HEADS = 16
IDX_DIM = 64
DSA_TOPK_MAX = 256

MOBA_HEADS = 8
MOBA_BLOCK = 256
MOBA_TOPK = 3
MOBA_Q_BLOCK = 32

DIL_HEADS = 8
DIL_PATTERNS = ((128, 1), (512, 4), (2048, 16))

D_FF = 4 * D_MODEL

EVEN_SPLITS = (MLA_Q_RANK, MLA_KV_RANK, MLA_ROPE, DSA_HEADS * HEAD_DIM, DSA_HEADS * HEAD_DIM, DSA_HEADS * HEAD_DIM, IDX_HEADS * IDX_DIM, IDX_DIM, IDX_HEADS)
EVEN_IN = sum(EVEN_SPLITS)
EVEN_OUT = MLA_HEADS * MLA_V + DSA_HEADS * HEAD_DIM
ODD_SPLITS = (MOBA_HEADS * HEAD_DIM,) * 3 + (DIL_HEADS * HEAD_DIM,) * 3
ODD_IN = sum(ODD_SPLITS)
ODD_OUT = (MOBA_HEADS + DIL_HEADS) * HEAD_DIM
N_EVEN = (DEPTH + 1) // 2
N_ODD = DEPTH // 2

kernel_name = 'hybrid_mla_dsa_moba_dilated'


def rmsnorm(x, g):
    xf = x.astype(jnp.float32)
    y = xf * lax.rsqrt(jnp.mean(xf * xf, axis=-1, keepdims=True) + NORM_EPS)
    return (y * g.astype(jnp.float32)).astype(x.dtype)


def rope_tables(seq, dim):
    inv = ROPE_THETA ** (-jnp.arange(0, dim, 2, dtype=jnp.float32) / dim)
    ang = jnp.arange(seq, dtype=jnp.float32)[:, None] * inv[None, :]
    return (jnp.cos(ang), jnp.sin(ang))


def apply_rope(x, rope):
    cos, sin = rope
    c = cos[None, :, None, :].astype(x.dtype)
    s = sin[None, :, None, :].astype(x.dtype)
    x1, x2 = jnp.split(x, 2, axis=-1)
    return jnp.concatenate([x1 * c - x2 * s, x1 * s + x2 * c], axis=-1)


def masked_softmax(scores, mask):
    return jax.nn.softmax(jnp.where(mask, scores.astype(jnp.float32), -jnp.inf), axis=-1)


def split_cols(x, sizes):
    offs = [int(o) for o in np.cumsum(sizes)[:-1]]
    return jnp.split(x, offs, axis=-1)


def sweep_query_blocks(fn, seq, block):
    n = seq // block
    out = jnp.moveaxis(lax.map(fn, jnp.arange(n)), 0, 1)
    return out.reshape((out.shape[0], n * block) + out.shape[3:])


def mla_attention(c_q, c_kv, k_pe, g_q, g_kv, w_uq, w_ukv, rope64):
    B, S, _ = c_q.shape
    q = (rmsnorm(c_q, g_q) @ w_uq).reshape(B, S, MLA_HEADS, MLA_NOPE + MLA_ROPE)
    kv = (rmsnorm(c_kv, g_kv) @ w_ukv).reshape(B, S, MLA_HEADS, MLA_NOPE + MLA_V)
    q_nope, q_pe = q[..., :MLA_NOPE], apply_rope(q[..., MLA_NOPE:], rope64)
    k_nope, v = kv[..., :MLA_NOPE], kv[..., MLA_NOPE:]
    k_pe = apply_rope(k_pe[:, :, None, :], rope64)[:, :, 0]
    scale = (MLA_NOPE + MLA_ROPE) ** -0.5
    pos = jnp.arange(S)

    def block(i):
        qs = i * Q_BLOCK
        qn = lax.dynamic_slice_in_dim(q_nope, qs, Q_BLOCK, axis=1)
        qp = lax.dynamic_slice_in_dim(q_pe, qs, Q_BLOCK, axis=1)
        s = jnp.einsum('bqhd,bkhd->bhqk', qn, k_nope) + jnp.einsum('bqhr,bkr->bhqk', qp, k_pe)
        qpos = qs + jnp.arange(Q_BLOCK)
        p = masked_softmax(s * scale, pos[None, :] <= qpos[:, None])
        return jnp.einsum('bhqk,bkhd->bqhd', p.astype(v.dtype), v)

    return sweep_query_blocks(block, S, Q_BLOCK)


def dsa_attention(q, k, v, q_idx, k_idx, w_idx, rope128, rope64):
    B, S = q.shape[:2]
    topk = min(DSA_TOPK_MAX, S // 4)
    q = apply_rope(q, rope128)
    k = apply_rope(k, rope128)
    q_idx = apply_rope(q_idx, rope64)
    k_idx = apply_rope(k_idx[:, :, None, :], rope64)[:, :, 0]
    w = w_idx.astype(jnp.float32) * IDX_HEADS ** -0.5
    pos = jnp.arange(S)
    gather = jax.vmap(lambda kb, ib: kb[ib])

    def block(i):
        qs = i * GATHER_Q_BLOCK
        qpos = qs + jnp.arange(GATHER_Q_BLOCK)
        qi = lax.dynamic_slice_in_dim(q_idx, qs, GATHER_Q_BLOCK, axis=1)
        wi = lax.dynamic_slice_in_dim(w, qs, GATHER_Q_BLOCK, axis=1)
        dots = jnp.einsum('bqhd,bsd->bqhs', qi, k_idx).astype(jnp.float32) * IDX_DIM ** -0.5
        score = jnp.einsum('bqh,bqhs->bqs', wi, jax.nn.relu(dots))
        causal = pos[None, :] <= qpos[:, None]
        score = jnp.where(causal[None], score, -jnp.inf)
        _, sel = lax.top_k(score, topk)
        valid = sel <= qpos[None, :, None]
        k_sel = gather(k, sel)
        v_sel = gather(v, sel)
        qb = lax.dynamic_slice_in_dim(q, qs, GATHER_Q_BLOCK, axis=1)
        s = jnp.einsum('bqhd,bqkhd->bhqk', qb, k_sel) * HEAD_DIM ** -0.5
        p = masked_softmax(s, valid[:, None])
        return jnp.einsum('bhqk,bqkhd->bqhd', p.astype(v.dtype), v_sel)

    return sweep_query_blocks(block, S, GATHER_Q_BLOCK)


def moba_attention(q, k, v, rope128):
    B, S, H, Dh = q.shape
    q = apply_rope(q, rope128)
    k = apply_rope(k, rope128)
    n_blk = -(-S // MOBA_BLOCK)
    s_pad = n_blk * MOBA_BLOCK
    pad = ((0, 0), (0, s_pad - S), (0, 0), (0, 0))
    q = jnp.transpose(jnp.pad(q, pad), (0, 2, 1, 3))
    k_blk = jnp.transpose(jnp.pad(k, pad), (0, 2, 1, 3)).reshape(B, H, n_blk, MOBA_BLOCK, Dh)
    v_blk = jnp.transpose(jnp.pad(v, pad), (0, 2, 1, 3)).reshape(B, H, n_blk, MOBA_BLOCK, Dh)
    k_mean = jnp.mean(k_blk, axis=3)
    n_sel = min(MOBA_TOPK, n_blk - 1)
    scale = Dh ** -0.5
    gather = jax.vmap(jax.vmap(lambda kb, ib: kb[ib]))

    def block(i):
        qs = i * MOBA_Q_BLOCK
        qpos = qs + jnp.arange(MOBA_Q_BLOCK)
        own = qs // MOBA_BLOCK
        qb = lax.dynamic_slice_in_dim(q, qs, MOBA_Q_BLOCK, axis=2)
        k_own = lax.dynamic_index_in_dim(k_blk, own, axis=2, keepdims=False)
        v_own = lax.dynamic_index_in_dim(v_blk, own, axis=2, keepdims=False)
        s_own = jnp.einsum('bhqd,bhkd->bhqk', qb, k_own) * scale
        own_mask = (own * MOBA_BLOCK + jnp.arange(MOBA_BLOCK))[None, :] <= qpos[:, None]
        own_mask = jnp.broadcast_to(own_mask, s_own.shape)
        if n_sel == 0:
            p = masked_softmax(s_own, own_mask)
            return jnp.einsum('bhqk,bhkd->bqhd', p.astype(v.dtype), v_own)
        gate = jnp.einsum('bhqd,bhnd->bhqn', qb, k_mean).astype(jnp.float32)
        gate = jnp.where(jnp.arange(n_blk) < own, gate, -jnp.inf)
        _, sel = lax.top_k(gate, n_sel)
        valid = jnp.repeat(sel < own, MOBA_BLOCK, axis=-1)
        k_sel = gather(k_blk, sel).reshape(B, H, MOBA_Q_BLOCK, n_sel * MOBA_BLOCK, Dh)
        v_sel = gather(v_blk, sel).reshape(B, H, MOBA_Q_BLOCK, n_sel * MOBA_BLOCK, Dh)
        s_sel = jnp.einsum('bhqd,bhqkd->bhqk', qb, k_sel) * scale
        p = masked_softmax(jnp.concatenate([s_sel, s_own], -1), jnp.concatenate([valid, own_mask], -1))
        p = p.astype(v.dtype)
        n_s = n_sel * MOBA_BLOCK
        return (jnp.einsum('bhqk,bhqkd->bqhd', p[..., :n_s], v_sel)
                + jnp.einsum('bhqk,bhkd->bqhd', p[..., n_s:], v_own))

    return sweep_query_blocks(block, s_pad, MOBA_Q_BLOCK)[:, :S]


def dilated_attention(q, k, v, rope128):
    B, S, H, Dh = q.shape
    q = apply_rope(q, rope128)
    k = apply_rope(k, rope128)
    scale = Dh ** -0.5

    def block(i):
        qs = i * GATHER_Q_BLOCK
        qpos = qs + jnp.arange(GATHER_Q_BLOCK)
        qb = lax.dynamic_slice_in_dim(q, qs, GATHER_Q_BLOCK, axis=1)
        outs, lses = [], []
        for window, dil in DIL_PATTERNS:
            idx = qpos[:, None] - dil * jnp.arange(window // dil + 1)[None, :]
            valid = idx >= 0
            idx = jnp.maximum(idx, 0)
            k_sel = k[:, idx]
            v_sel = v[:, idx]
            s = jnp.einsum('bqhd,bqnhd->bhqn', qb, k_sel).astype(jnp.float32) * scale
            s = jnp.where(valid[None, None], s, -jnp.inf)
            lse = jax.nn.logsumexp(s, axis=-1)
            p = jnp.exp(s - lse[..., None]).astype(v.dtype)
            outs.append(jnp.einsum('bhqn,bqnhd->bqhd', p, v_sel))
            lses.append(lse)
        wts = jax.nn.softmax(jnp.stack(lses, 0), axis=0)
        out = jnp.einsum('gbhq,gbqhd->bqhd', wts, jnp.stack(outs, 0).astype(jnp.float32))
        return out.astype(q.dtype)

    return sweep_query_blocks(block, S, GATHER_Q_BLOCK)


def even_mixer(h, w_in, g_q, g_kv, w_uq, w_ukv, w_out, rope128, rope64):
    B, S, _ = h.shape
    c_q, c_kv, k_pe, q, k, v, q_idx, k_idx, w_idx = split_cols(h @ w_in, EVEN_SPLITS)
    a = mla_attention(c_q, c_kv, k_pe, g_q, g_kv, w_uq, w_ukv, rope64)
    shp = (B, S, DSA_HEADS, HEAD_DIM)
    b = dsa_attention(q.reshape(shp), k.reshape(shp), v.reshape(shp),
                      q_idx.reshape(B, S, IDX_HEADS, IDX_DIM), k_idx, w_idx, rope128, rope64)
    o = jnp.concatenate([a.reshape(B, S, -1), b.reshape(B, S, -1)], axis=-1)
    return o @ w_out


def odd_mixer(h, w_in, w_out, rope128):
    B, S, _ = h.shape
    qc, kc, vc, qd, kd, vd = split_cols(h @ w_in, ODD_SPLITS)
    sc = (B, S, MOBA_HEADS, HEAD_DIM)
    sd = (B, S, DIL_HEADS, HEAD_DIM)
    c = moba_attention(qc.reshape(sc), kc.reshape(sc), vc.reshape(sc), rope128)
    d = dilated_attention(qd.reshape(sd), kd.reshape(sd), vd.reshape(sd), rope128)
    o = jnp.concatenate([c.reshape(B, S, -1), d.reshape(B, S, -1)], axis=-1)
    return o @ w_out


def squared_relu_mlp(h, w1, w2):
    return jnp.square(jax.nn.relu(h @ w1)) @ w2


def setup_inputs(seed: int = 0) -> dict:
    key = jax.random.key(seed)
    ks = jax.random.split(key, 14)

    def nrm(k, shape, fan_in):
        return jax.random.normal(k, shape, jnp.float32) * fan_in ** -0.5

    def gain(k, shape):
        return 1.0 + 0.02 * jax.random.normal(k, shape, jnp.float32)

    return {
        'x': jax.random.normal(ks[0], (BATCH, SEQ, D_MODEL), jnp.float32),
        'ln_mix': gain(ks[1], (DEPTH, D_MODEL)),
        'ln_mlp': gain(ks[2], (DEPTH, D_MODEL)),
        'ln_final': gain(ks[3], (D_MODEL,)),
        'e_w_in': nrm(ks[4], (N_EVEN, D_MODEL, EVEN_IN), D_MODEL),
        'e_g_q': gain(ks[5], (N_EVEN, MLA_Q_RANK)),
        'e_g_kv': gain(ks[6], (N_EVEN, MLA_KV_RANK)),
        'e_w_uq': nrm(ks[7], (N_EVEN, MLA_Q_RANK, MLA_HEADS * (MLA_NOPE + MLA_ROPE)), MLA_Q_RANK),
        'e_w_ukv': nrm(ks[8], (N_EVEN, MLA_KV_RANK, MLA_HEADS * (MLA_NOPE + MLA_V)), MLA_KV_RANK),
        'e_w_out': nrm(ks[9], (N_EVEN, EVEN_OUT, D_MODEL), EVEN_OUT),
        'o_w_in': nrm(ks[10], (N_ODD, D_MODEL, ODD_IN), D_MODEL),
        'o_w_out': nrm(ks[11], (N_ODD, ODD_OUT, D_MODEL), ODD_OUT),
        'mlp_w1': nrm(ks[12], (DEPTH, D_MODEL, D_FF), D_MODEL),
        'mlp_w2': nrm(ks[13], (DEPTH, D_FF, D_MODEL), D_FF),
    }


def reference(x, ln_mix, ln_mlp, ln_final, e_w_in, e_g_q, e_g_kv, e_w_uq, e_w_ukv, e_w_out,
              o_w_in, o_w_out, mlp_w1, mlp_w2):
    S = x.shape[1]
    rope128 = rope_tables(S, HEAD_DIM)
    rope64 = rope_tables(S, MLA_ROPE)
    for layer in range(DEPTH):
        j = layer // 2
        h = rmsnorm(x, ln_mix[layer])
        if layer % 2 == 0:
            x = x + even_mixer(h, e_w_in[j], e_g_q[j], e_g_kv[j], e_w_uq[j], e_w_ukv[j], e_w_out[j], rope128, rope64)
        else:
            x = x + odd_mixer(h, o_w_in[j], o_w_out[j], rope128)
        h = rmsnorm(x, ln_mlp[layer])
        x = x + squared_relu_mlp(h, mlp_w1[layer], mlp_w2[layer])
    return rmsnorm(x, ln_final)
```

```python
import numpy as np
import ml_dtypes
from contextlib import ExitStack
import concourse.bass as bass
import concourse.mybir as mybir
from concourse.bass_utils import run_bass_kernel_spmd

F32 = mybir.dt.float32
BF16 = mybir.dt.bfloat16
AF = mybir.ActivationFunctionType
ALU = mybir.AluOpType
AX = mybir.AxisListType

D = 2048
T = 2048
NT = 16
SEQ = 8192
EPS = 1e-6
NEG = -30000.0
EVEN_IN = 5008
ODD_IN = 6144
DFF = 8192

ENGS = ("pe", "act", "dve", "pool", "sp")
DEBUG_SCRATCH = False


class Sched:
    def __init__(self, nc, ndma=12):
        self.nc = nc
        self.q = {e: [] for e in ENGS}
        self.cnt = {e: 0 for e in ENGS}
        self.seen = {e: {} for e in ENGS}
        self.last_w = {}
        self.readers = {}
        self.sems = {}
        self.ndma = ndma
        self.dma_n = {e: 0 for e in ENGS}
        self.ncc = 0
        self.ninst = 0

    def sem_names(self):
        names = ["s_" + e for e in ("pe", "act", "dve", "pool")]
        for e in ("sp", "pool", "act"):
            for i in range(self.ndma):
                names.append("d_%s_%d" % (e, i))
        for i in range(4):
            names.append("d_cc%d" % i)
        return names

    def _deps(self, eng, reads, writes):
        deps = {}

        def add(ev):
            if ev is None:
                return
            s, v = ev
            if deps.get(s, 0) < v:
                deps[s] = v
        for r in reads:
            add(self.last_w.get(r))
        for w in writes:
            add(self.last_w.get(w))
            for s, v in self.readers.get(w, {}).items():
                add((s, v))
        out = []
        for s, v in deps.items():
            if s == "s_" + eng and eng == "pe":
                continue
            if self.seen[eng].get(s, 0) >= v:
                continue
            self.seen[eng][s] = v
            out.append((s, v))
        return out

    def _record(self, ev, reads, writes):
        s, v = ev
        for w in writes:
            self.last_w[w] = ev
            self.readers[w] = {}
        for r in reads:
            d = self.readers.setdefault(r, {})
            if d.get(s, 0) < v:
                d[s] = v

    def op(self, eng, fn, reads=(), writes=()):
        waits = self._deps(eng, reads, writes)
        self.cnt[eng] += 1
        ev = ("s_" + eng, self.cnt[eng])
        self.q[eng].append((waits, fn, ev))
        self._record(ev, reads, writes)
        self.ninst += 1
        return ev

    def dma(self, eng, out, in_, reads=(), writes=()):
        waits = self._deps(eng, reads, writes)
        n = self.dma_n[eng]
        self.dma_n[eng] += 1
        s = "d_%s_%d" % (eng, n % self.ndma)
        prev = 16 * (n // self.ndma)
        if prev > 0 and self.seen[eng].get(s, 0) < prev:
            waits.append((s, prev))
            self.seen[eng][s] = prev
        ev = (s, prev + 16)
        fn = lambda e, out=out, in_=in_: e.dma_start(out=out, in_=in_)
        self.q[eng].append((waits, fn, ev))
        self._record(ev, reads, writes)
        self.ninst += 1
        return ev

    def collective(self, fn, reads, writes):
        waits = self._deps("pool", reads, writes)
        ev = ("d_cc%d" % self.ncc, 1)
        self.ncc += 1
        self.q["pool"].append((waits, fn, ev))
        self._record(ev, reads, writes)
        return ev

    def barrier(self):
        evs = [("s_" + e, self.cnt[e]) for e in ("pe", "act", "dve", "pool") if self.cnt[e] > 0]
        for e in ("sp", "pool", "act"):
            n = self.dma_n[e]
            for i in range(min(n, self.ndma)):
                last = ((n - 1 - i) // self.ndma) * self.ndma + i
                evs.append(("d_%s_%d" % (e, i), 16 * (last // self.ndma + 1)))
        for i in range(self.ncc):
            evs.append(("d_cc%d" % i, 1))
        for eng in ENGS:
            waits = []
            for s, v in evs:
                if self.seen[eng].get(s, 0) >= v:
                    continue
                self.seen[eng][s] = v
                waits.append((s, v))
            self.q[eng].append((waits, None, None))
        self.last_w = {}
        self.readers = {}

    def emit(self, block):
        SM = self.sems

        def run(e, engobj):
            for waits, fn, ev in self.q[e]:
                for s, v in waits:
                    engobj.wait_ge(SM[s], v)
                if fn is None:
                    continue
                ins = fn(engobj)
                if ev[0].startswith("d_cc"):
                    ins.then_inc(SM[ev[0]])
                else:
                    ins.then_inc(SM[ev[0]], 16 if ev[0].startswith("d_") else 1)

        @block.tensor
        def _(pe):
            run("pe", pe)

        @block.scalar
        def _(act):
            run("act", act)

        @block.vector
        def _(dve):
            run("dve", dve)

        @block.gpsimd
        def _(pool):
            run("pool", pool)

        @block.sync
        def _(sp):
            run("sp", sp)


QE_QTN = 0
QE_QTPE = 1024
QE_QT = 1536
QE_QIDX = 2560
QE_ROWS = 3584
KE_KTN = 0
KE_KTPE = 1024
KE_KT = 1088
KE_KIDX = 2112
KE_VM = 2176
KE_VD = 3200
KE_ROWS = 4224
QO_QTM = 0
QO_QTD = 1024
QO_ROWS = 2048
KO_KTM = 0
KO_KTD = 1024
KO_VM = 2048
KO_VD = 3072
KO_ROWS = 4096


class Prog:
    def __init__(self, parts):
        self.parts = parts
        self.nc = bass.Bass("TRN2", target_bir_lowering=False)
        self.S = Sched(self.nc)
        self.es = ExitStack()
        self.dram = {}
        self.ext_in = []
        self.ext_out = []
        self.uid = 0

    def din(self, name, shape, dt=F32):
        if name not in self.dram:
            self.dram[name] = self.nc.dram_tensor(name, list(shape), dt, kind="ExternalInput")
            self.ext_in.append(name)
        return self.dram[name]

    def dten(self, name, shape, dt, producer, consumers):
        if name in self.dram:
            return self.dram[name]
        pin = producer in self.parts
        cin = all(c in self.parts for c in consumers)
        if pin and cin:
            kind = "Internal"
        elif pin:
            kind = "ExternalOutput"
            self.ext_out.append(name)
        else:
            kind = "ExternalInput"
            self.ext_in.append(name)
        self.dram[name] = self.nc.dram_tensor(name, list(shape), dt, kind=kind)
        return self.dram[name]

    def dscratch(self, name, shape, dt):
        if name not in self.dram:
            if DEBUG_SCRATCH:
                self.dram[name] = self.nc.dram_tensor(name, list(shape), dt, kind="ExternalOutput")
                self.ext_out.append(name)
            else:
                self.dram[name] = self.nc.dram_tensor(name, list(shape), dt, kind="Internal")
        return self.dram[name]

    def setup(self):
        nc, es = self.nc, self.es
        self.ARENA_BYTES = 176 * 1024
        self.arena = es.enter_context(nc.sbuf_tensor("arena", [128, self.ARENA_BYTES // 2], BF16))
        self.pers = es.enter_context(nc.sbuf_tensor("pers", [128, 24 * 1024 // 2], BF16))
        self.psf = [es.enter_context(nc.psum_tensor("psf%d" % i, [128, 512], F32))[:] for i in range(6)]
        self.pst = [es.enter_context(nc.psum_tensor("pst%d" % i, [128, 1024], BF16))[:] for i in range(2)]
        for nm in self.S.sem_names():
            self.S.sems[nm] = es.enter_context(nc.semaphore(nm))
        self.block = es.enter_context(nc.Block())
        self.pers_off = 0
        self.ident = self.pv([128, 128], BF16)
        self.cos128 = self.pv([128, NT, 64], F32)
        self.sin128 = self.pv([128, NT, 64], F32)
        self.cos64 = self.pv([128, NT, 32], F32)
        self.sin64 = self.pv([128, NT, 32], F32)
        self.gbc = self.pv([128, D], F32)
        self.ss = self.pv([128, NT], F32)
        self.rstd = self.pv([128, NT], F32)
        self.small = self.pv([128, 64], F32)
        S = self.S
        idd = self.din("ident", [128, 128])
        S.dma("pool", self.ident, idd.ap(), writes=["ident"])
        for nm, t, w in (("cos128", self.cos128, 64), ("sin128", self.sin128, 64), ("cos64", self.cos64, 32), ("sin64", self.sin64, 32)):
            dd = self.din(nm, [T, w])
            S.dma("sp", t, dd.ap().rearrange("(k p) c -> p k c", p=128), writes=[nm])

    def _view(self, base, off, shape, dt):
        esz = 4 if dt == F32 else 2
        n = int(np.prod(shape[1:]))
        assert off % 4 == 0
        v = base[:, off // 2: off // 2 + n * esz // 2]
        if dt == F32:
            v = v.bitcast(F32)
        if len(shape) == 3:
            v = v.rearrange("p (a b) -> p a b", b=shape[2])
        elif len(shape) == 4:
            v = v.rearrange("p (a b c) -> p a b c", b=shape[2], c=shape[3])
        if shape[0] < 128:
            v = v[0:shape[0]]
        return v

    def pv(self, shape, dt):
        esz = 4 if dt == F32 else 2
        nbytes = int(np.prod(shape[1:])) * esz
        nbytes = (nbytes + 3) // 4 * 4
        v = self._view(self.pers, self.pers_off, shape, dt)
        self.pers_off += nbytes
        assert self.pers_off <= 24 * 1024
        return v

    def av(self, off_kib, shape, dt):
        off = int(off_kib * 1024)
        esz = 4 if dt == F32 else 2
        assert off + int(np.prod(shape[1:])) * esz <= self.ARENA_BYTES, (off_kib, shape)
        return self._view(self.arena, off, shape, dt)

    def key(self, base):
        self.uid += 1
        return "%s#%d" % (base, self.uid)

    def load_gain(self, gname, n):
        g = self.din(gname, [n])
        self.S.dma("sp", self.gbc[:, 0:n], g.ap().partition_broadcast(128), writes=["gbc"])

    def rstd_from_ss(self, col, n):
        S = self.S
        ss, rstd = self.ss, self.rstd
        S.op("dve", lambda e: e.tensor_scalar(rstd[:, col:col + 1], ss[:, col:col + 1], 1.0 / n, EPS, ALU.mult, ALU.add),
             reads=["ss%d" % col], writes=["rstd%d" % col])
        S.op("act", lambda e: e.activation(rstd[:, col:col + 1], rstd[:, col:col + 1], AF.Ln), reads=["rstd%d" % col], writes=["rstd%d" % col])
        S.op("act", lambda e: e.activation(rstd[:, col:col + 1], rstd[:, col:col + 1], AF.Exp, scale=-0.5), reads=["rstd%d" % col], writes=["rstd%d" % col])

    def build_actT(self, src, src_dt, actT, xs, hb, norm_gain=None, ncols=D, tiles=range(NT), src_keys=()):
        S = self.S
        KC = ncols // 128
        junk = xs
        for k in tiles:
            b = k % 2
            xk = xs[b]
            if src_dt == F32:
                S.dma("sp", xk[:, 0:ncols], src(k), reads=list(src_keys), writes=["xs%d" % b])
                if norm_gain is not None:
                    hbk = hb[b]
                    S.op("act", lambda e, xk=xk, hbk=hbk, k=k: e.activation(hbk[:, 0:ncols], xk[:, 0:ncols], AF.Square, accum_out=self.ss[:, k:k + 1]),
                         reads=["xs%d" % b], writes=["hb%d" % b, "ss%d" % k])
                    self.rstd_from_ss(k, ncols)
                    S.op("dve", lambda e, xk=xk, hbk=hbk, k=k: e.scalar_tensor_tensor(hbk[:, 0:ncols], xk[:, 0:ncols], self.rstd[:, k:k + 1], self.gbc[:, 0:ncols], ALU.mult, ALU.mult),
                         reads=["xs%d" % b, "rstd%d" % k, "gbc"], writes=["hb%d" % b])
                else:
                    hbk = hb[b]
                    S.op("dve", lambda e, xk=xk, hbk=hbk: e.tensor_copy(hbk[:, 0:ncols], xk[:, 0:ncols]), reads=["xs%d" % b], writes=["hb%d" % b])
                srcT = hb[b]
                skey = "hb%d" % b
            else:
                hbk = hb[b]
                S.dma("sp", hbk[:, 0:ncols], src(k), reads=list(src_keys), writes=["hb%d" % b])
                srcT = hbk
                skey = "hb%d" % b
            for g0 in range(0, KC, 8):
                ng = min(8, KC - g0)
                pt = self.pst[(g0 // 8) % 2]
                pk = "pst%d" % ((g0 // 8) % 2)
                for c in range(ng):
                    S.op("pe", lambda e, pt=pt, c=c, g0=g0, srcT=srcT: e.transpose(pt[:, c * 128:(c + 1) * 128], srcT[:, (g0 + c) * 128:(g0 + c + 1) * 128], self.ident),
                         reads=[skey, "ident"], writes=[pk])
                dst = actT[:, g0:g0 + ng, k * 128:(k + 1) * 128]
                srcp = pt[:, 0:ng * 128].rearrange("p (a b) -> p a b", b=128)
                eng = "act" if (g0 // 8) % 2 == 0 else "dve"
                if eng == "act":
                    S.op("act", lambda e, dst=dst, srcp=srcp: e.copy(dst, srcp), reads=[pk], writes=["actT%d" % k])
                else:
                    S.op("dve", lambda e, dst=dst, srcp=srcp: e.tensor_copy(dst, srcp), reads=[pk], writes=["actT%d" % k])

    def load_w(self, wt, wkey, w, r0, KC, c0, ncols):
        S = self.S
        wap = w.ap()
        for c4 in range(0, KC, 4):
            n4 = min(4, KC - c4)
            src = wap[r0 + c4 * 128: r0 + (c4 + n4) * 128, c0:c0 + ncols].rearrange("(c p) n -> p c n", p=128)
            S.dma("pool", wt[:, c4:c4 + n4, 0:ncols], src, writes=[wkey + "_%d" % c4])
        return [wkey + "_%d" % c4 for c4 in range(0, KC, 4)]

    def linear_tm(self, actT, KC, w, blocks, wt, tiles=range(NT), r0=0, akey="actT"):
        S = self.S
        nb = len(blocks)
        wkeys = {}
        wkeys[0] = self.load_w(wt[0], "wt0", w, r0, KC, blocks[0][0], blocks[0][1])
        bi = 0
        for i, (c0, ncols, handler, fin) in enumerate(blocks):
            if i + 1 < nb:
                wkeys[i + 1] = self.load_w(wt[(i + 1) % 2], "wt%d" % ((i + 1) % 2), w, r0, KC, blocks[i + 1][0], blocks[i + 1][1])
            wti = wt[i % 2]
            for k in tiles:
                ps = self.psf[bi % 4]
                pk = "psf%d" % (bi % 4)
                bi += 1
                for c in range(KC):
                    S.op("pe", lambda e, ps=ps, c=c, k=k, wti=wti, ncols=ncols: e.matmul(ps[:, 0:ncols], actT[:, c, k * 128:(k + 1) * 128], wti[:, c, 0:ncols], start=(c == 0), stop=(c == KC - 1)),
                         reads=["%s%d" % (akey, k)] + wkeys[i], writes=[pk])
                handler(k, ps, pk)
            if fin is not None:
                fin()

    def make_handler(self, bufs, ncols, pieces, touts, vouts, stage_T, stage_V, extra=None):
        S = self.S
        ysb2, yb2, tmp = bufs
        state = {"i": 0}
        only_copy = all(p[0] == "copy" for p in pieces)

        def handler(k, ps, pk):
            i = state["i"] % 2
            state["i"] += 1
            ysb, yb = ysb2[i], yb2[i]
            yk, ybk = "ysb%d" % i, "yb%d" % i
            if extra is not None:
                extra(k, ps, pk)
            if only_copy:
                for (_, so, n, do, _, _, _) in pieces:
                    S.op("act", lambda e, so=so, n=n, do=do: e.copy(yb[:, do:do + n], ps[:, so:so + n]), reads=[pk], writes=[ybk])
            else:
                S.op("act", lambda e: e.copy(ysb[:, 0:ncols], ps[:, 0:ncols]), reads=[pk], writes=[yk])
                for (kind, so, n, do, H, Dh, tabs) in pieces:
                    if kind == "copy":
                        S.op("pool", lambda e, so=so, n=n, do=do: e.tensor_copy(yb[:, do:do + n], ysb[:, so:so + n]), reads=[yk], writes=[ybk])
                    else:
                        hd = Dh // 2
                        cosT, sinT = tabs
                        xv = ysb[:, so:so + n].rearrange("p (h t d) -> p h t d", h=H, t=2)
                        ov = yb[:, do:do + n].rearrange("p (h t d) -> p h t d", h=H, t=2)
                        x1, x2 = xv[:, :, 0, :], xv[:, :, 1, :]
                        o1, o2 = ov[:, :, 0, :], ov[:, :, 1, :]
                        cb = cosT[:, k, :].unsqueeze(1).to_broadcast([128, H, hd])
                        sb_ = sinT[:, k, :].unsqueeze(1).to_broadcast([128, H, hd])
                        t = [tmp[j][:, 0:H * hd].rearrange("p (h d) -> p h d", h=H) for j in range(4)]
                        S.op("dve", lambda e, x1=x1, cb=cb, t=t: e.tensor_tensor(t[0], x1, cb, ALU.mult), reads=[yk, "tabs"], writes=["tmp0"])
                        S.op("pool", lambda e, x2=x2, sb_=sb_, t=t: e.tensor_tensor(t[1], x2, sb_, ALU.mult), reads=[yk, "tabs"], writes=["tmp1"])
                        S.op("dve", lambda e, x1=x1, sb_=sb_, t=t: e.tensor_tensor(t[2], x1, sb_, ALU.mult), reads=[yk, "tabs"], writes=["tmp2"])
                        S.op("pool", lambda e, x2=x2, cb=cb, t=t: e.tensor_tensor(t[3], x2, cb, ALU.mult), reads=[yk, "tabs"], writes=["tmp3"])
                        S.op("dve", lambda e, o1=o1, t=t: e.tensor_tensor(o1, t[0], t[1], ALU.subtract), reads=["tmp0", "tmp1"], writes=[ybk])
                        S.op("pool", lambda e, o2=o2, t=t: e.tensor_tensor(o2, t[2], t[3], ALU.add), reads=["tmp2", "tmp3"], writes=[ybk])
            for t0 in range(0, len(touts), 8):
                grp = touts[t0:t0 + 8]
                pt = self.pst[(t0 // 8) % 2]
                ptk = "pst%d" % ((t0 // 8) % 2)
                for ci, (off, g) in enumerate(grp):
                    S.op("pe", lambda e, pt=pt, ci=ci, off=off: e.transpose(pt[:, ci * 128:(ci + 1) * 128], yb[:, off:off + 128], self.ident), reads=[ybk, "ident"], writes=[ptk])
                g0 = grp[0][1]
                assert [g for _, g in grp] == list(range(g0, g0 + len(grp)))
                dst = stage_T[:, g0:g0 + len(grp), k * 128:(k + 1) * 128]
                srcp = pt[:, 0:len(grp) * 128].rearrange("p (a b) -> p a b", b=128)
                S.op("dve", lambda e, dst=dst, srcp=srcp: e.tensor_copy(dst, srcp), reads=[ptk], writes=["stageT"])
            for (off, n, so) in vouts:
                S.op("pool", lambda e, off=off, n=n, so=so: e.tensor_copy(stage_V[:, k, so:so + n], yb[:, off:off + n]), reads=[ybk], writes=["stageV"])
        return handler

    def phase_A(self, layer):
        S = self.S
        even = (layer % 2 == 0)
        xin = self.x_tensor(layer)
        QROWS, KROWS = (QE_ROWS, KE_ROWS) if even else (QO_ROWS, KO_ROWS)
        qs = self.dten("qs%d" % layer, [QROWS, T], BF16, "A%d" % layer, ["B%d" % layer])
        kv = self.dten("kvown%d" % layer, [KROWS, T], BF16, "A%d" % layer, ["G%d" % layer])
        S.barrier()
        actT = self.av(0, [128, 16, T], BF16)
        wt = [self.av(64, [128, 16, 512], BF16), self.av(80, [128, 16, 512], BF16)]
        xs = [self.av(96, [128, D], F32), self.av(104, [128, D], F32)]
        hb = [self.av(112, [128, D], BF16), self.av(116, [128, D], BF16)]
        stage = [self.av(96, [128, 8192], BF16), self.av(112, [128, 8192], BF16)]
        ysb2 = [self.av(128, [128, 512], F32), self.av(130, [128, 512], F32)]
        yb2 = [self.av(132, [128, 512], BF16), self.av(133, [128, 512], BF16)]
        tmp = [self.av(134 + i, [128, 256], F32) for i in range(4)]
        bufs = (ysb2, yb2, tmp)
        cqT = self.av(138, [128, 4, T], BF16)
        ckvT = self.av(154, [128, 2, T], BF16)
        self.load_gain("ln_mix%d" % layer, D)
        xap = xin.ap()
        self.build_actT(lambda k: xap[k * 128:(k + 1) * 128, :], F32, actT, xs, hb, norm_gain=True)
        S.barrier()
        r128 = (self.cos128, self.sin128)
        r64 = (self.cos64, self.sin64)
        sidx = [0]

        def stg():
            sidx[0] += 1
            return stage[sidx[0] % 2]

        def fin_T(st, dests):
            def f():
                for (g, p0, npart, dt_, row0) in dests:
                    S.dma("sp", dt_.ap()[row0:row0 + npart, :], st.rearrange("p (g t) -> p g t", t=T)[p0:p0 + npart, g, :], reads=["stageT"], writes=[self.key("dr")])
            return f

        def fin_V(st, dt_, row0, c0, n, width=1024):
            def f():
                dst = bass.AP(dt_, row0 * T + c0, [[width, 128], [128 * width, NT], [1, n]])
                S.dma("sp", dst, st.rearrange("p (k c) -> p k c", c=512)[:, :, 0:n], reads=["stageV"], writes=[self.key("dr")])
            return f

        def T_block(c0, ncols, rope, dests_fn):
            st = stg()
            stT = st.rearrange("p (g t) -> p g t", t=T)
            if rope is None:
                pieces = [("copy", 0, ncols, 0, 0, 0, None)]
            else:
                H, Dh, tabs = rope
                pieces = [("rope", 0, ncols, 0, H, Dh, tabs)]
            ng = ncols // 128
            touts = [(g * 128, g) for g in range(ng)]
            h = self.make_handler(bufs, ncols, pieces, touts, [], stT, None)
            return (c0, ncols, h, fin_T(st, dests_fn()))

        def V_block(c0, ncols, dt_, row0, vc0):
            st = stg()
            stV = st.rearrange("p (k c) -> p k c", c=512)
            h = self.make_handler(bufs, ncols, [("copy", 0, ncols, 0, 0, 0, None)], [], [(0, ncols, 0)], None, stV)
            return (c0, ncols, h, fin_V(st, dt_, row0, vc0, ncols))

        if even:
            w_in = self.din("e_w_in", [D, EVEN_IN])
            ws = self.dten("ws0", [T, 16], F32, "A0", ["B0"])
            self.load_gain("e_g_q", 512)
            gq_done = [False]

            def lat_handler(n, dstT, gname, ssbase):
                def h(k, ps, pk):
                    junk = ysb2[0]
                    col = k
                    S.op("act", lambda e: e.activation(junk[:, 0:n], ps[:, 0:n], AF.Square, accum_out=self.ss[:, col:col + 1]), reads=[pk], writes=["ysb0", "ss%d" % col])
                    self.rstd_from_ss(col, n)
                    yb = yb2[0]
                    S.op("dve", lambda e: e.scalar_tensor_tensor(yb[:, 0:n], ps[:, 0:n], self.rstd[:, col:col + 1], self.gbc[:, 0:n], ALU.mult, ALU.mult),
                         reads=[pk, "rstd%d" % col, "gbc"], writes=["yb0"])
                    pt = self.pst[0]
                    for c in range(n // 128):
                        S.op("pe", lambda e, c=c: e.transpose(pt[:, c * 128:(c + 1) * 128], yb[:, c * 128:(c + 1) * 128], self.ident), reads=["yb0", "ident"], writes=["pst0"])
                    S.op("dve", lambda e: e.tensor_copy(dstT[:, :, k * 128:(k + 1) * 128], pt[:, 0:n].rearrange("p (a b) -> p a b", b=128)), reads=["pst0"], writes=[gname + "%d" % k])
                return h

            blocks = []
            blocks.append((0, 512, lat_handler(512, cqT, "cqT", 0), lambda: self.load_gain("e_g_kv", 256)))
            st_kpe = stg()
            stT_kpe = st_kpe.rearrange("p (g t) -> p g t", t=T)
            h_ckv = lat_handler(256, ckvT, "ckvT", 0)
            h_kpe = self.make_handler(bufs, 320, [("rope", 256, 64, 0, 1, 64, r64), ("copy", 0, 64, 64, 0, 0, None)], [(0, 0)], [], stT_kpe, None)

            def h_b2(k, ps, pk):
                h_ckv(k, ps, pk)
                h_kpe(k, ps, pk)
            blocks.append((512, 320, h_b2, fin_T(st_kpe, [(0, 0, 64, kv, KE_KTPE)])))
            for i in range(2):
                blocks.append(T_block(832 + i * 512, 512, (4, 128, r128), lambda i=i: [(g, 0, 128, qs, QE_QT + (i * 4 + g) * 128) for g in range(4)]))
            for i in range(2):
                blocks.append(T_block(1856 + i * 512, 512, (4, 128, r128), lambda i=i: [(g, 0, 128, kv, KE_KT + (i * 4 + g) * 128) for g in range(4)]))
            for i in range(2):
                blocks.append(V_block(2880 + i * 512, 512, kv, KE_VD, i * 512))
            for i in range(2):
                def dests(i=i):
                    d = []
                    for g in range(4):
                        d.append((g, 0, 64, qs, QE_QIDX + (i * 8 + 2 * g) * 64))
                        d.append((g, 64, 64, qs, QE_QIDX + (i * 8 + 2 * g + 1) * 64))
                    return d
                blocks.append(T_block(3904 + i * 512, 512, (8, 64, r64), dests))
            st_ki = stg()
            stT_ki = st_ki.rearrange("p (g t) -> p g t", t=T)
            wsb = self.av(162, [128, NT, 16], F32)

            def ex_w(k, ps, pk):
                S.op("act", lambda e: e.activation(wsb[:, k, :], ps[:, 64:80], AF.Copy, scale=0.03125), reads=[pk], writes=["wsb"])
            h_ki = self.make_handler(bufs, 80, [("rope", 0, 64, 0, 1, 64, r64), ("copy", 0, 64, 64, 0, 0, None)], [(0, 0)], [], stT_ki, None, extra=ex_w)

            def fin_ki():
                fin_T(st_ki, [(0, 0, 64, kv, KE_KIDX)])()
                S.dma("sp", ws.ap().rearrange("(k p) c -> p k c", p=128), wsb, reads=["wsb"], writes=[self.key("dr")])
            blocks.append((4928, 80, h_ki, fin_ki))
            self.linear_tm(actT, 16, w_in, blocks, wt)
            w_uq = self.din("e_w_uq", [512, 1536])
            blocks = []
            for i in range(4):
                st = stg()
                stT = st.rearrange("p (g t) -> p g t", t=T)
                pieces = [("copy", 0, 128, 0, 0, 0, None), ("copy", 192, 128, 128, 0, 0, None),
                          ("rope", 128, 64, 256, 1, 64, r64), ("rope", 320, 64, 320, 1, 64, r64)]
                h = self.make_handler(bufs, 384, pieces, [(0, 0), (128, 1), (256, 2)], [], stT, None)
                dests = [(0, 0, 128, qs, QE_QTN + (2 * i) * 128), (1, 0, 128, qs, QE_QTN + (2 * i + 1) * 128),
                         (2, 0, 64, qs, QE_QTPE + (2 * i) * 64), (2, 64, 64, qs, QE_QTPE + (2 * i + 1) * 64)]
                blocks.append((i * 384, 384, h, fin_T(st, dests)))
            self.linear_tm(cqT, 4, w_uq, blocks, wt, akey="cqT")
            w_ukv = self.din("e_w_ukv", [256, 2048])
            blocks = []
            for i in range(4):
                st = stg()
                stT = st.rearrange("p (g t) -> p g t", t=T)
                st2 = stg()
                stV = st2.rearrange("p (k c) -> p k c", c=512)
                pieces = [("copy", 0, 128, 0, 0, 0, None), ("copy", 256, 128, 128, 0, 0, None),
                          ("copy", 128, 128, 256, 0, 0, None), ("copy", 384, 128, 384, 0, 0, None)]
                h = self.make_handler(bufs, 512, pieces, [(0, 0), (128, 1)], [(256, 256, 0)], stT, stV)
                f1 = fin_T(st, [(0, 0, 128, kv, KE_KTN + (2 * i) * 128), (1, 0, 128, kv, KE_KTN + (2 * i + 1) * 128)])
                f2 = fin_V(st2, kv, KE_VM, i * 256, 256)

                def f(f1=f1, f2=f2):
                    f1()
                    f2()
                blocks.append((i * 512, 512, h, f))
            self.linear_tm(ckvT, 2, w_ukv, blocks, wt, akey="ckvT")
        else:
            w_in = self.din("o_w_in", [D, ODD_IN])
            blocks = []
            for i in range(2):
                blocks.append(T_block(0 + i * 512, 512, (4, 128, r128), lambda i=i: [(g, 0, 128, qs, QO_QTM + (i * 4 + g) * 128) for g in range(4)]))
            for i in range(2):
                blocks.append(T_block(1024 + i * 512, 512, (4, 128, r128), lambda i=i: [(g, 0, 128, kv, KO_KTM + (i * 4 + g) * 128) for g in range(4)]))
            for i in range(2):
                blocks.append(V_block(2048 + i * 512, 512, kv, KO_VM, i * 512))
            for i in range(2):
                blocks.append(T_block(3072 + i * 512, 512, (4, 128, r128), lambda i=i: [(g, 0, 128, qs, QO_QTD + (i * 4 + g) * 128) for g in range(4)]))
            for i in range(2):
                blocks.append(T_block(4096 + i * 512, 512, (4, 128, r128), lambda i=i: [(g, 0, 128, kv, KO_KTD + (i * 4 + g) * 128) for g in range(4)]))
            for i in range(2):
                blocks.append(V_block(5120 + i * 512, 512, kv, KO_VD, i * 512))
            self.linear_tm(actT, 16, w_in, blocks, wt)
        S.barrier()

    def x_tensor(self, layer):
        if layer == 0:
            return self.din("x0", [T, D])
        return self.dten("x%d" % layer, [T, D], F32, "B%d" % (layer - 1), ["A%d" % layer, "B%d" % layer])

    def phase_G(self, layer):
        S = self.S
        KROWS = KE_ROWS if layer % 2 == 0 else KO_ROWS
        kv = self.dten("kvown%d" % layer, [KROWS, T], BF16, "A%d" % layer, ["G%d" % layer])
        kva = self.dten("kvall%d" % layer, [4 * KROWS, T], BF16, "G%d" % layer, ["B%d" % layer])
        S.barrier()
        S.collective(lambda e: e.collective_compute("AllGather", ALU.bypass, replica_groups=[[0, 1, 2, 3], [4, 5, 6, 7]], ins=[kv.ap()], outs=[kva.ap()]), ["kvown"], ["kvall"])
        S.barrier()

    def attention(self, name, kva, KROWS, qs, kt_row0, v_row0, q_row0, o_d, o_col0, scale, chunks, bias_fn,
                  pe_rows=None, pre_head=None, pre_tile=None, nheads=8):
        S = self.S
        KT = [self.av(0, [128, 4, T], BF16), self.av(16, [128, 4, T], BF16)]
        V = [self.av(32, [128, 64, 130], BF16), self.av(48.5, [128, 64, 130], BF16)]
        QT = [self.av(65, [128, T], BF16), self.av(69, [128, T], BF16)]
        KTpe = self.av(73, [64, 4, T], BF16)
        QTpe = [self.av(89, [64, T], BF16), self.av(93, [64, T], BF16)]
        P = [self.av(97 + i, [128, 512], BF16) for i in range(3)]
        ost = [self.av(100, [128, NT, 128], BF16), self.av(104, [128, NT, 128], BF16)]
        rec = self.small
        for b in range(2):
            S.op("pool", lambda e, b=b: e.memset(V[b][:, :, 128:130], 1.0), writes=["Vone%d" % b])
        if pe_rows is not None:
            krow, qrow = pe_rows
            S.dma("sp", KTpe, bass.AP(kva, krow * T, [[T, 64], [KROWS * T, 4], [1, T]]), writes=["KTpe"])

        def load_head(h):
            b = h % 2
            S.dma("sp", KT[b], bass.AP(kva, (kt_row0 + h * 128) * T, [[T, 128], [KROWS * T, 4], [1, T]]), writes=["KT%d" % b])
            for r in range(4):
                src = bass.AP(kva, (r * KROWS + v_row0) * T + h * 128, [[1024, 128], [128 * 1024, NT], [1, 128]])
                S.dma("sp", V[b][:, r * NT:(r + 1) * NT, 0:128], src, writes=["V%d_%d" % (b, r)])
            S.dma("sp", QT[b], qs.ap()[q_row0 + h * 128: q_row0 + (h + 1) * 128, :], writes=["QT%d" % b])
            if pe_rows is not None:
                S.dma("sp", QTpe[b], qs.ap()[qrow + h * 64: qrow + (h + 1) * 64, :], writes=["QTpe%d" % b])

        load_head(0)
        sc = 0
        oc = 0
        for h in range(nheads):
            b = h % 2
            if h + 1 < nheads:
                load_head(h + 1)
            hk = ["KT%d" % b, "QT%d" % b] + (["KTpe", "QTpe%d" % b] if pe_rows is not None else [])
            vk = ["V%d_%d" % (b, r) for r in range(4)] + ["Vone%d" % b]
            if pre_head is not None:
                pre_head(h, KT[b], QT[b], "KT%d" % b, "QT%d" % b)
            for k in range(NT):
                if pre_tile is not None:
                    pre_tile(h, k, QT[b], "QT%d" % b)
                cl = chunks(k)
                O = self.psf[3 + oc % 2]
                ok = "psf%d" % (3 + oc % 2)
                oc += 1
                for ci, kk in enumerate(cl):
                    Sps = self.psf[sc % 3]
                    sk = "psf%d" % (sc % 3)
                    Pb = P[sc % 3]
                    pk = "P%d" % (sc % 3)
                    sc += 1
                    for m in range(4):
                        bl = bias_fn(h, k, kk, m)
                        nmm = 1 + (1 if pe_rows is not None else 0) + len(bl)
                        out = Sps[:, m * 128:(m + 1) * 128]
                        j = 0
                        S.op("pe", lambda e, out=out, m=m, kk=kk, k=k, b=b, last=(nmm == 1): e.matmul(out, KT[b][:, m, kk * 128:(kk + 1) * 128], QT[b][:, k * 128:(k + 1) * 128], start=True, stop=last),
                             reads=hk, writes=[sk])
                        j += 1
                        if pe_rows is not None:
                            S.op("pe", lambda e, out=out, m=m, kk=kk, k=k, b=b, last=(j + 1 == nmm): e.matmul(out, KTpe[:, m, kk * 128:(kk + 1) * 128], QTpe[b][:, k * 128:(k + 1) * 128], start=False, stop=last),
                                 reads=hk, writes=[sk])
                            j += 1
                        for (lhsT, rhs, rk) in bl:
                            S.op("pe", lambda e, out=out, lhsT=lhsT, rhs=rhs, last=(j + 1 == nmm): e.matmul(out, lhsT, rhs, start=False, stop=last), reads=rk, writes=[sk])
                            j += 1
                    S.op("act", lambda e, Pb=Pb, Sps=Sps: e.activation(Pb, Sps, AF.Exp, scale=scale), reads=[sk], writes=[pk])
                    for m in range(4):
                        S.op("pe", lambda e, O=O, Pb=Pb, m=m, kk=kk, b=b, first=(ci == 0 and m == 0), last=(ci == len(cl) - 1 and m == 3):
                             e.matmul(O[:, 0:129], Pb[:, m * 128:(m + 1) * 128], V[b][:, m * NT + kk, 0:129], start=first, stop=last), reads=[pk] + vk, writes=[ok])
                S.op("dve", lambda e, O=O: e.reciprocal(rec[:, 0:1], O[:, 128:129]), reads=[ok], writes=["rec"])
                S.op("dve", lambda e, O=O, k=k, b=b: e.tensor_scalar(ost[b][:, k, :], O[:, 0:128], rec[:, 0:1], 0.0, ALU.mult, ALU.add), reads=[ok, "rec"], writes=["ost%d" % b])
            dst = bass.AP(o_d, o_col0 + h * 128, [[D, 128], [128 * D, NT], [1, 128]])
            S.dma("sp", dst, ost[b], reads=["ost%d" % b], writes=[self.key("od")])

    def load_const(self, name, shape, view, key):
        d = self.din(name, shape)
        self.S.dma("pool", view, d.ap(), writes=[key])

    def phase_B(self, layer, final):
        S = self.S
        even = (layer % 2 == 0)
        KROWS = KE_ROWS if even else KO_ROWS
        QROWS = QE_ROWS if even else QO_ROWS
        xin = self.x_tensor(layer)
        qs = self.dten("qs%d" % layer, [QROWS, T], BF16, "A%d" % layer, ["B%d" % layer])
        kva = self.dten("kvall%d" % layer, [4 * KROWS, T], BF16, "G%d" % layer, ["B%d" % layer])
        o_d = self.dscratch("o%d" % layer, [T, D], BF16)
        x2_d = self.dscratch("x2_%d" % layer, [T, D], F32)
        if final:
            xout = self.dscratch("x3_%d" % layer, [T, D], F32)
        else:
            xout = self.x_tensor(layer + 1)
        S.barrier()
        ident = self.ident
        dense = lambda k: list(range(k + 1))
        if even:
            cz = self.av(108, [128, 4, 128], BF16)
            self.load_const("causalT", [128, 4, 128], cz, "causalT")

            def bias_mla(h, k, kk, m):
                if kk == k:
                    return [(ident, cz[:, m, :], ["ident", "causalT"])]
                return []
            self.attention("mla", kva, KROWS, qs, KE_KTN, KE_VM, QE_QTN, o_d, 0, 192 ** -0.5, dense, bias_mla, pe_rows=(KE_KTPE, QE_QTPE))
            S.barrier()
            mb_d = self.dscratch("mb_d", [NT, 128, SEQ], BF16)
            ws = self.dten("ws0", [T, 16], F32, "A0", ["B0"])
            kidx = self.av(0, [64, 4, T], BF16)
            Ia = self.av(16, [128, SEQ], F32)
            Ib = self.av(48, [128, SEQ], F32)
            work = self.av(80, [128, SEQ], F32)
            rr = [self.av(112, [128, 512], F32), self.av(114, [128, 512], F32)]
            qidx = [self.av(116, [64, 16, 128], BF16), self.av(120, [64, 16, 128], BF16)]
            mbo = [self.av(124, [128, SEQ], BF16), self.av(140, [128, SEQ], BF16)]
            wsb = self.av(156, [128, NT, 16], F32)
            dcb = self.av(157, [128, 512], F32)
            m8 = self.av(159, [128, 8], F32)
            thr = self.av(159.25, [128, 1], F32)
            S.dma("sp", kidx, bass.AP(kva, KE_KIDX * T, [[T, 64], [KROWS * T, 4], [1, T]]), writes=["kidx"])
            S.dma("sp", wsb, ws.ap().rearrange("(k p) c -> p k c", p=128), writes=["wsb"])
            self.load_const("dsa_causal", [128, 512], dcb, "dcb")
            ri = 0
            for k in range(NT):
                qb = qidx[k % 2]
                qk = "qidx%d" % (k % 2)
                S.dma("sp", qb, bass.AP(qs, QE_QIDX * T + k * 128, [[T, 64], [64 * T, 16], [1, 128]]), writes=[qk])
                L = (k + 1) * 512
                for kk in range(k + 1):
                    sl = slice(kk * 512, (kk + 1) * 512)
                    for hh in range(16):
                        ps = self.psf[hh % 3]
                        pk = "psf%d" % (hh % 3)
                        S.op("pe", lambda e, ps=ps, qb=qb, hh=hh, kk=kk: e.matmul(ps.rearrange("p (a b) -> p a b", b=128), qb[:, hh, :], kidx[:, :, kk * 128:(kk + 1) * 128], start=True, stop=True), reads=[qk, "kidx"], writes=[pk])
                        r = rr[ri % 2]
                        rk = "rr%d" % (ri % 2)
                        ri += 1
                        S.op("act", lambda e, r=r, ps=ps: e.activation(r, ps, AF.Relu), reads=[pk], writes=[rk])
                        acc, ak, eng = (Ia, "Ia", "dve") if hh % 2 == 0 else (Ib, "Ib", "pool")
                        wcol = wsb[:, k, hh:hh + 1]
                        if hh < 2:
                            S.op(eng, lambda e, acc=acc, r=r, wcol=wcol, sl=sl: e.tensor_scalar(acc[:, sl], r, wcol, 0.0, ALU.mult, ALU.add), reads=[rk, "wsb"], writes=[ak])
                        elif eng == "dve":
                            S.op(eng, lambda e, acc=acc, r=r, wcol=wcol, sl=sl: e.scalar_tensor_tensor(acc[:, sl], r, wcol, acc[:, sl], ALU.mult, ALU.add), reads=[rk, "wsb", ak], writes=[ak])
                        else:
                            S.op(eng, lambda e, r=r, wcol=wcol: e.tensor_scalar(r, r, wcol, 0.0, ALU.mult, ALU.add), reads=[rk, "wsb"], writes=[rk])
                            S.op(eng, lambda e, acc=acc, r=r, sl=sl: e.tensor_tensor(acc[:, sl], acc[:, sl], r, ALU.add), reads=[rk, ak], writes=[ak])
                S.op("dve", lambda e, L=L: e.tensor_tensor(Ia[:, 0:L], Ia[:, 0:L], Ib[:, 0:L], ALU.add), reads=["Ia", "Ib"], writes=["Ia"])
                S.op("dve", lambda e, L=L: e.tensor_tensor(Ia[:, L - 512:L], Ia[:, L - 512:L], dcb, ALU.add), reads=["Ia", "dcb"], writes=["Ia"])
                cur = Ia
                ck = "Ia"
                for it in range(32):
                    S.op("dve", lambda e, cur=cur, L=L: e.max(out=m8, in_=cur[:, 0:L]), reads=[ck], writes=["m8"])
                    if it < 31:
                        S.op("dve", lambda e, cur=cur, L=L: e.match_replace(out=work[:, 0:L], in_to_replace=m8, in_values=cur[:, 0:L], imm_value=-3.0e38), reads=[ck, "m8"], writes=["work"])
                        cur = work
                        ck = "work"
                S.op("dve", lambda e: e.tensor_scalar(thr, m8[:, 7:8], -1.0e29, 0.0, ALU.max, ALU.add), reads=["m8"], writes=["thr"])
                mo = mbo[k % 2]
                mk = "mbo%d" % (k % 2)
                S.op("pool", lambda e, mo=mo, L=L: e.tensor_scalar(mo[:, 0:L], Ia[:, 0:L], thr[:, 0:1], NEG, ALU.is_lt, ALU.mult), reads=["Ia", "thr"], writes=[mk])
                S.dma("sp", mb_d.ap()[k, :, 0:L], mo[:, 0:L], reads=[mk], writes=["mb_d%d" % k])
            S.barrier()
            mbi = [self.av(134, [128, SEQ], BF16), self.av(150, [128, SEQ], BF16)]
            cnt = [0]
            cur_mb = [None, None]

            def pre_tile_dsa(h, k, QTb, qk):
                i = cnt[0] % 2
                cnt[0] += 1
                L = (k + 1) * 512
                S.dma("sp", mbi[i][:, 0:L], mb_d.ap()[k, :, 0:L], reads=["mb_d%d" % k], writes=["mbi%d" % i])
                cur_mb[0], cur_mb[1] = mbi[i], "mbi%d" % i

            def bias_dsa(h, k, kk, m):
                mbt, mk = cur_mb
                c0 = kk * 512 + m * 128
                return [(mbt[:, c0:c0 + 128], ident, [mk, "ident"])]
            self.attention("dsa", kva, KROWS, qs, KE_KT, KE_VD, QE_QT, o_d, 1024, 128 ** -0.5, dense, bias_dsa, pre_tile=pre_tile_dsa)
            S.barrier()
        else:
            cz = self.av(108, [128, 4, 128], BF16)
            self.load_const("moba_causalT", [128, 4, 128], cz, "mcz")
            E = self.av(110, [32, 32 * 128], BF16)
            self.load_const("esel", [32, 32 * 128], E, "E")
            pastm = self.av(118, [128, NT, 32], F32)
            fix2 = self.av(120, [128, NT, 32], F32)
            self.load_const("pastmask", [128, NT, 32], pastm, "pastm")
            self.load_const("fix2", [128, NT, 32], fix2, "fix2")
            tsum = self.av(122, [128, 64], F32)
            km = self.av(122.25, [128, NT, 2], BF16)
            gm = self.av(122.5, [128, 32], F32)
            m8 = self.av(122.75, [128, 8], F32)
            thr = self.av(123, [128, 1], F32)
            selb = self.av(123.25, [128, 32], F32)
            selbb = self.av(123.5, [128, 32], BF16)
            selbT = [self.av(124, [32, 128], BF16), self.av(124.25, [32, 128], BF16)]
            cnt = [0]
            cur = [None, None]

            def pre_head_moba(h, KTb, QTb, kk_, qk_):
                S.op("dve", lambda e: e.tensor_reduce(out=tsum, in_=KTb.rearrange("p r (k t) -> p (r k) t", t=128), axis=AX.X, op=ALU.add), reads=[kk_], writes=["tsum"])
                ts = tsum.rearrange("p (r k) -> p r k", r=4)
                for r2 in range(2):
                    S.op("dve", lambda e, r2=r2: e.tensor_tensor(km[:, :, r2], ts[:, 2 * r2, :], ts[:, 2 * r2 + 1, :], ALU.add), reads=["tsum"], writes=["km"])

            def pre_tile_moba(h, k, QTb, qk):
                i = cnt[0] % 2
                cnt[0] += 1
                pg = self.psf[5]
                S.op("pe", lambda e: e.matmul(pg[:, 0:32], QTb[:, k * 128:(k + 1) * 128], km.rearrange("p k r -> p (k r)"), start=True, stop=True), reads=[qk, "km"], writes=["psf5"])
                S.op("dve", lambda e: e.tensor_tensor(gm, pg[:, 0:32], pastm[:, k, :], ALU.add), reads=["psf5", "pastm"], writes=["gm"])
                S.op("dve", lambda e: e.max(out=m8, in_=gm), reads=["gm"], writes=["m8"])
                S.op("dve", lambda e: e.tensor_scalar(thr, m8[:, 2:3], -1.0e29, 0.0, ALU.max, ALU.add), reads=["m8"], writes=["thr"])
                S.op("dve", lambda e: e.tensor_scalar(selb, gm, thr[:, 0:1], -NEG, ALU.is_ge, ALU.mult), reads=["gm", "thr"], writes=["selb"])
                S.op("dve", lambda e: e.tensor_tensor(selbb, selb, fix2[:, k, :], ALU.add), reads=["selb", "fix2"], writes=["selbb"])
                pt = self.pst[0]
                S.op("pe", lambda e: e.transpose(pt[0:32, 0:128], selbb, ident), reads=["selbb", "ident"], writes=["pst0"])
                S.op("act", lambda e, i=i: e.copy(selbT[i], pt[0:32, 0:128]), reads=["pst0"], writes=["selbT%d" % i])
                cur[0], cur[1] = selbT[i], "selbT%d" % i

            def bias_moba(h, k, kk, m):
                n = 2 * kk + m // 2
                bl = [(E[:, n * 128:(n + 1) * 128], cur[0], ["E", cur[1]])]
                if kk == k:
                    bl.append((ident, cz[:, m, :], ["ident", "mcz"]))
                return bl
            self.attention("moba", kva, KROWS, qs, KO_KTM, KO_VM, QO_QTM, o_d, 0, 128 ** -0.5, dense, bias_moba, pre_head=pre_head_moba, pre_tile=pre_tile_moba)
            S.barrier()
            dhi = self.av(108, [128, 20, 128], BF16)
            dlo = self.av(113, [128, 20, 128], BF16)
            self.load_const("dil_hi", [128, 20, 128], dhi, "dhi")
            self.load_const("dil_lo", [128, 20, 128], dlo, "dlo")

            def bias_dil(h, k, kk, m):
                t = (k - kk) * 4 + m
                return [(ident, dhi[:, t, :], ["ident", "dhi"]), (ident, dlo[:, t, :], ["ident", "dlo"])]
            self.attention("dil", kva, KROWS, qs, KO_KTD, KO_VD, QO_QTD, o_d, 1024, 128 ** -0.5, lambda k: list(range(max(0, k - 4), k + 1)), bias_dil)
            S.barrier()
        actT = self.av(0, [128, 16, T], BF16)
        wt = [self.av(64, [128, 16, 512], BF16), self.av(80, [128, 16, 512], BF16)]
        xs = [self.av(96, [128, D], F32), self.av(104, [128, D], F32)]
        hb = [self.av(112, [128, D], BF16), self.av(116, [128, D], BF16)]
        oap = o_d.ap()
        self.build_actT(lambda k: oap[k * 128:(k + 1) * 128, :], BF16, actT, xs, hb)
        S.barrier()
        w_out = self.din("e_w_out" if even else "o_w_out", [D, D])
        xr = [self.av(96 + 2 * i, [128, 512], F32) for i in range(2)]
        xo = [self.av(100 + 2 * i, [128, 512], F32) for i in range(2)]
        xap = xin.ap()
        x2ap = x2_d.ap()
        st = {"i": 0}

        def res_handler(src_ap, dst_ap, c0, xr, xo):
            def h(k, ps, pk):
                i = st["i"] % 2
                st["i"] += 1
                S.dma("sp", xr[i], src_ap[k * 128:(k + 1) * 128, c0:c0 + 512], writes=["xr%d" % i])
                S.op("dve", lambda e, i=i: e.tensor_tensor(xo[i], ps, xr[i], ALU.add), reads=[pk, "xr%d" % i], writes=["xo%d" % i])
                S.dma("pool", dst_ap[k * 128:(k + 1) * 128, c0:c0 + 512], xo[i], reads=["xo%d" % i], writes=[self.key("x2w")])
            return h
        self.linear_tm(actT, 16, w_out, [(c * 512, 512, res_handler(xap, x2ap, c * 512, xr, xo), None) for c in range(4)], wt)
        S.barrier()
        self.load_gain("ln_mlp%d" % layer, D)
        HT = self.av(96, [128, 64, 512], BF16)
        xs = [self.av(96, [128, D], F32), self.av(104, [128, D], F32)]
        hb = [self.av(112, [128, D], BF16), self.av(116, [128, D], BF16)]
        self.build_actT(lambda k: x2ap[k * 128:(k + 1) * 128, :], F32, actT, xs, hb, norm_gain=True)
        S.barrier()
        w1 = self.din("mlp_w1_%d" % layer, [D, DFF])
        w2 = self.din("mlp_w2_%d" % layer, [DFF, D])
        xr = [self.av(160 + 2 * i, [128, 512], F32) for i in range(2)]
        xo = [self.av(164 + 2 * i, [128, 512], F32) for i in range(2)]
        rl = [self.av(168 + 2 * i, [128, 512], F32) for i in range(2)]
        xoap = xout.ap()
        wi = 0
        nw = 0
        seq = []
        for tg in range(4):
            for hb_ in range(16):
                seq.append(("w1", tg, hb_))
            for cb in range(4):
                for kg in range(4):
                    seq.append(("w2", tg, cb, kg))

        def issue(idx):
            it = seq[idx]
            b = idx % 2
            if it[0] == "w1":
                return self.load_w(wt[b], "wt%d" % b, w1, 0, 16, it[2] * 512, 512)
            return self.load_w(wt[b], "wt%d" % b, w2, it[3] * 2048, 16, it[2] * 512, 512)
        wk = {0: issue(0)}
        bi = 0
        ri = 0
        for idx, it in enumerate(seq):
            if idx + 1 < len(seq):
                wk[idx + 1] = issue(idx + 1)
            wti = wt[idx % 2]
            tg = it[1]
            if it[0] == "w1":
                hb_ = it[2]
                for sub in range(4):
                    ps = self.psf[bi % 4]
                    pk = "psf%d" % (bi % 4)
                    bi += 1
                    for c in range(16):
                        S.op("pe", lambda e, ps=ps, c=c, sub=sub, tg=tg, wti=wti: e.matmul(ps, wti[:, c, sub * 128:(sub + 1) * 128], actT[:, c, tg * 512:(tg + 1) * 512], start=(c == 0), stop=(c == 15)),
                             reads=["actT%d" % (tg * 4 + q) for q in range(4)] + wk[idx], writes=[pk])
                    r = rl[ri % 2]
                    rk = "rl%d" % (ri % 2)
                    ri += 1
                    S.op("act", lambda e, r=r, ps=ps: e.activation(r, ps, AF.Relu), reads=[pk], writes=[rk])
                    hc = hb_ * 4 + sub
                    S.op("pool" if ri % 2 else "dve", lambda e, r=r, hc=hc: e.tensor_tensor(HT[:, hc, :], r, r, ALU.mult), reads=[rk], writes=["HT%d" % hc])
            else:
                cb, kg = it[2], it[3]
                for tt in range(4):
                    ps = self.psf[tt]
                    pk = "psf%d" % tt
                    for c in range(16):
                        hc = kg * 16 + c
                        S.op("pe", lambda e, ps=ps, c=c, hc=hc, tt=tt, wti=wti, kg=kg: e.matmul(ps, HT[:, hc, tt * 128:(tt + 1) * 128], wti[:, c, :], start=(kg == 0 and c == 0), stop=(kg == 3 and c == 15)),
                             reads=["HT%d" % hc] + wk[idx], writes=[pk])
                    if kg == 3:
                        k = tg * 4 + tt
                        res_handler(x2ap, xoap, cb * 512, xr, xo)(k, ps, pk)
        S.barrier()
        if final:
            y = self.nc.dram_tensor("y", [T, D], F32, kind="ExternalOutput")
            self.dram["y"] = y
            self.ext_out.append("y")
            self.load_gain("ln_final", D)
            xs = [self.av(96, [128, D], F32), self.av(104, [128, D], F32)]
            yo = [self.av(112, [128, D], F32), self.av(120, [128, D], F32)]
            junk = self.av(128, [128, D], F32)
            for k in range(NT):
                b = k % 2
                S.dma("sp", xs[b], xoap[k * 128:(k + 1) * 128, :], writes=["xs%d" % b])
                S.op("act", lambda e, b=b, k=k: e.activation(junk, xs[b], AF.Square, accum_out=self.ss[:, k:k + 1]), reads=["xs%d" % b], writes=["junk", "ss%d" % k])
                self.rstd_from_ss(k, D)
                S.op("dve", lambda e, b=b, k=k: e.scalar_tensor_tensor(yo[b], xs[b], self.rstd[:, k:k + 1], self.gbc, ALU.mult, ALU.mult), reads=["xs%d" % b, "rstd%d" % k, "gbc"], writes=["yo%d" % b])
                S.dma("pool", y.ap()[k * 128:(k + 1) * 128, :], yo[b], reads=["yo%d" % b], writes=["y%d" % k])
            S.barrier()

    def build(self):
        self.setup()
        for p in self.parts:
            layer = int(p[1])
            if p[0] == "A":
                self.phase_A(layer)
            elif p[0] == "G":
                self.phase_G(layer)
            else:
                self.phase_B(layer, final=(layer == 1))
        self.S.barrier()
        self.S.emit(self.block)
        self.es.close()
        return self.nc


def _rope_tables(pos, dim):
    inv = (10000.0 ** (-np.arange(0, dim, 2, dtype=np.float32) / dim)).astype(np.float32)
    ang = pos.astype(np.float32)[:, None] * inv[None, :]
    return np.cos(ang).astype(np.float32), np.sin(ang).astype(np.float32)


def _core_consts(j):
    c = {}
    pos = np.concatenate([np.arange(128) + (4 * k + j) * 128 for k in range(NT)])
    c["cos128"], c["sin128"] = _rope_tables(pos, 128)
    c["cos64"], c["sin64"] = _rope_tables(pos, 64)
    c["ident"] = np.eye(128, dtype=np.float32)
    ki = np.arange(128)[:, None]
    qi = np.arange(128)[None, :]
    tri = np.where(ki <= qi, 0.0, NEG).astype(np.float32)
    cz = np.zeros((128, 4, 128), np.float32)
    mz = np.zeros((128, 4, 128), np.float32)
    for m in range(4):
        cz[:, m, :] = 0.0 if m < j else (tri if m == j else NEG)
        if m // 2 == j // 2:
            mz[:, m, :] = 0.0 if m < j else (tri if m == j else NEG)
        elif m // 2 > j // 2:
            mz[:, m, :] = NEG
    c["causalT"] = cz
    c["moba_causalT"] = mz
    dc = np.zeros((128, 4, 128), np.float32)
    for m in range(4):
        dc[:, m, :] = 0.0 if m < j else (np.where(qi.T >= ki.T, 0.0, -1.0e30) if m == j else -1.0e30)
    c["dsa_causal"] = dc.reshape(128, 512)
    es = np.zeros((32, 32, 128), np.float32)
    for n in range(32):
        es[n, n, :] = 1.0
    c["esel"] = es.reshape(32, 32 * 128)
    pm = np.zeros((128, NT, 32), np.float32)
    f2 = np.zeros((128, NT, 32), np.float32)
    for k in range(NT):
        own = (4 * k + j) // 2
        pm[:, k, own:] = -1.0e30
        f2[:, k, :own] = NEG
    c["pastmask"] = pm
    c["fix2"] = f2
    hi = np.zeros((128, 20, 128), np.float32)
    lo = np.zeros((128, 20, 128), np.float32)
    for dk in range(5):
        for m in range(4):
            delta = 4 * dk + j - m
            d = delta * 128 + qi - ki
            mult = ((d >= 0) & (d <= 128)).astype(np.float64) + ((d >= 0) & (d <= 512) & (d % 4 == 0)) + ((d >= 0) & (d <= 2048) & (d % 16 == 0))
            lb = np.where(mult > 0, np.log(np.maximum(mult, 1.0)) * np.sqrt(128.0), NEG)
            h_ = lb.astype(ml_dtypes.bfloat16).astype(np.float64)
            hi[:, dk * 4 + m, :] = h_
            lo[:, dk * 4 + m, :] = np.where(mult > 0, lb - h_, 0.0)
    c["dil_hi"] = hi
    c["dil_lo"] = lo
    return c


_PROG_CACHE = {}


def _get_prog(parts):
    key = tuple(parts)
    if key not in _PROG_CACHE:
        p = Prog(list(parts))
        p.build()
        _PROG_CACHE[key] = p
    return _PROG_CACHE[key]


def _weights(inp):
    w = {}
    w["ln_mix0"], w["ln_mix1"] = inp["ln_mix"][0], inp["ln_mix"][1]
    w["ln_mlp0"], w["ln_mlp1"] = inp["ln_mlp"][0], inp["ln_mlp"][1]
    w["ln_final"] = inp["ln_final"]
    w["e_w_in"] = inp["e_w_in"][0]
    w["e_g_q"] = inp["e_g_q"][0]
    w["e_g_kv"] = inp["e_g_kv"][0]
    w["e_w_uq"] = inp["e_w_uq"][0]
    w["e_w_ukv"] = inp["e_w_ukv"][0]
    w["e_w_out"] = inp["e_w_out"][0]
    w["o_w_in"] = inp["o_w_in"][0]
    w["o_w_out"] = inp["o_w_out"][0]
    for l in range(2):
        w["mlp_w1_%d" % l] = inp["mlp_w1"][l]
        w["mlp_w2_%d" % l] = inp["mlp_w2"][l]
    return {k: np.ascontiguousarray(np.asarray(v, dtype=np.float32)) for k, v in w.items()}


def _shard_x(x):
    x = np.asarray(x, dtype=np.float32)
    out = []
    for c in range(8):
        b, j = c // 4, c % 4
        xb = x[b].reshape(16, 4, 128, D)[:, j]
        out.append(np.ascontiguousarray(xb.reshape(T, D)))
    return out


def _unshard(ys):
    out = np.zeros((2, SEQ, D), np.float32)
    for c in range(8):
        b, j = c // 4, c % 4
        out[b].reshape(16, 4, 128, D)[:, j] = np.asarray(ys[c], dtype=np.float32).reshape(16, 128, D)
    return out


def run_parts(parts, per_core_extra):
    prog = _get_prog(parts)
    in_maps = []
    for c in range(8):
        src = per_core_extra[c]
        m = {}
        for name in prog.ext_in:
            m[name] = src[name]
        in_maps.append(m)
    res = run_bass_kernel_spmd(prog.nc, in_maps, core_ids=list(range(8)))
    return [{n: r[n] for n in prog.ext_out} for r in res.results]


FUSED = False


def kernel(**inputs):
    w = _weights(inputs)
    xs = _shard_x(inputs["x"])
    base = []
    for c in range(8):
        d = dict(w)
        d.update(_core_consts(c % 4))
        d["x0"] = xs[c]
        base.append(d)
    if FUSED:
        outs = run_parts(("A0", "G0", "B0", "A1", "G1", "B1"), base)
        return _unshard([o["y"] for o in outs])
    for layer in range(2):
        oa = run_parts(("A%d" % layer,), base)
        for c in range(8):
            base[c].update({k: v for k, v in oa[c].items() if k != "kvown%d" % layer})
            g = (c // 4) * 4
            base[c]["kvall%d" % layer] = np.concatenate([np.asarray(oa[g + r]["kvown%d" % layer]) for r in range(4)], axis=0)
        ob = run_parts(("B%d" % layer,), base)
        for c in range(8):
            base[c].update(ob[c])
    return _unshard([base[c]["y"] for c in range(8)])
```
